# Optimizing a Trainium2 kernel written in Bass

```python
import math
import jax
import jax.numpy as jnp
from jax import lax
import numpy as np


D_MODEL = 2048
BATCH = 2
SEQ = 8192
DEPTH = 4

CTX_LEN = 256
GRID_W = 64
GROUP_W = D_MODEL // 4
HG_DK = 128
HG_HEADS = GROUP_W // HG_DK
HG_W = HG_HEADS * HG_DK
SSD_HEADDIM = 64
SSD_HEADS = GROUP_W // SSD_HEADDIM
SSD_W = SSD_HEADS * SSD_HEADDIM
SSD_GROUPS = 2
SSD_STATE = 128
SSD_CONV = 4
SSD_CONV_DIM = SSD_W + 2 * SSD_GROUPS * SSD_STATE
LRU_W = GROUP_W
LRU_BW = 64
LRU_BLOCKS = LRU_W // LRU_BW
LRU_C = 8.0
LRU_CONV = 4
ML_DH = 128
ML_HEADS = GROUP_W // ML_DH
ML_W = ML_HEADS * ML_DH
MIX_W = HG_W + SSD_W + LRU_W + ML_W
HG_COLS = 5 * HG_W
SSD_COLS = SSD_W + SSD_CONV_DIM + 2 * SSD_HEADS
LRU_COLS = 2 * LRU_W
ML_COLS = 4 * ML_W + 4 * ML_HEADS
IN_COLS = HG_COLS + SSD_COLS + LRU_COLS + ML_COLS
CHUNK = 64
N_EXPERT_GROUPS = 4
EXPERTS_PER_GROUP = 8
N_EXPERTS = N_EXPERT_GROUPS * EXPERTS_PER_GROUP
TOP_K = 2
D_EXPERT = D_MODEL // 4
MOE_BLOCK = 256
NORM_EPS = 1e-6
MASK_NEG = -1e30
STATE_NEG = -1e30

kernel_name = 'hybrid_parallel_heads_diffusion_block'


def rmsnorm(x, g):
    xf = x.astype(jnp.float32)
    y = xf * lax.rsqrt(jnp.mean(xf * xf, axis=-1, keepdims=True) + NORM_EPS)
    return (y * g.astype(jnp.float32)).astype(x.dtype)


def head_rms(x):
    return x * lax.rsqrt(jnp.mean(x * x, axis=-1, keepdims=True) + NORM_EPS)


def to_heads(t, h):
    b, l, w = t.shape
    return t.reshape(b, l, h, w // h).transpose(0, 2, 1, 3)


def conv_centred(x, w):
    k, ch = w.shape
    lo = k // 2
    return lax.conv_general_dilated(x, w[:, None, :].astype(x.dtype), window_strides=(1,),
                                    padding=[(lo, k - 1 - lo)],
                                    dimension_numbers=('NWC', 'WIO', 'NWC'),
                                    feature_group_count=ch)


def chunked_gla(q, k, v, log_f, s0):
    bn, h, l, kd = q.shape
    vd = v.shape[-1]
    n = l // CHUNK
    vec = log_f.shape[-1] != 1
    causal = jnp.tril(jnp.ones((CHUNK, CHUNK), bool))

    def chunks(t):
        return jnp.moveaxis(t.reshape(bn, h, n, CHUNK, t.shape[-1]), 2, 0)

    def step(s, inp):
        qc, kc, vc, lc = inp
        b = jnp.cumsum(lc, axis=2)
        diff = b[:, :, :, None, :] - b[:, :, None, :, :]
        dec = jnp.exp(jnp.where(causal[:, :, None], diff, MASK_NEG))
        if vec:
            att = jnp.einsum('bhtk,bhtsk,bhsk->bhts', qc, dec, kc)
        else:
            att = jnp.einsum('bhtk,bhsk->bhts', qc, kc) * dec[..., 0]
        o = (jnp.einsum('bhts,bhsv->bhtv', att, vc)
             + jnp.einsum('bhtk,bhkv->bhtv', qc * jnp.exp(b), s))
        bl = b[:, :, -1:, :]
        s = (s * jnp.exp(bl)[:, :, 0, :, None]
             + jnp.einsum('bhsk,bhsv->bhkv', kc * jnp.exp(bl - b), vc))
        return s, o

    s, o = lax.scan(step, s0, (chunks(q), chunks(k), chunks(v), chunks(log_f)))
    return jnp.moveaxis(o, 0, 2).reshape(bn, h, l, vd), s


def chunked_mlstm(q, k, v, log_i, log_f, state):
    bn, h, l, kd = q.shape
    n = l // CHUNK
    causal = jnp.tril(jnp.ones((CHUNK, CHUNK), bool))

    def chunks(t):
        return jnp.moveaxis(t.reshape(bn, h, n, CHUNK, *t.shape[3:]), 2, 0)

    def step(carry, inp):
        cm, nv, m = carry
        qc, kc, vc, ic, fc = inp
        b = jnp.cumsum(fc, axis=-1)
        dmat = jnp.where(causal, b[..., :, None] - b[..., None, :] + ic[..., None, :], MASK_NEG)
        g = b + m[..., None]
        mt = jnp.maximum(g, jnp.max(dmat, axis=-1))
        w_intra = jnp.exp(dmat - mt[..., None])
        w_inter = jnp.exp(g - mt)
        s = jnp.einsum('bhtk,bhsk->bhts', qc, kc) * w_intra
        num = (jnp.einsum('bhts,bhsv->bhtv', s, vc)
               + w_inter[..., None] * jnp.einsum('bhtk,bhkv->bhtv', qc, cm))
        den = jnp.sum(s, axis=-1) + w_inter * jnp.einsum('bhtk,bhk->bht', qc, nv)
        hout = num / jnp.maximum(jnp.abs(den), jnp.exp(-mt))[..., None]
        bl = b[..., -1]
        dl = bl[..., None] - b + ic
        m_new = jnp.maximum(bl + m, jnp.max(dl, axis=-1))
        ws = jnp.exp(dl - m_new[..., None])
        wc = jnp.exp(bl + m - m_new)
        cm = wc[..., None, None] * cm + jnp.einsum('bhs,bhsk,bhsv->bhkv', ws, kc, vc)
        nv = wc[..., None] * nv + jnp.einsum('bhs,bhsk->bhk', ws, kc)
        return (cm, nv, m_new), hout

    state, hs = lax.scan(step, state, tuple(chunks(t) for t in (q, k, v, log_i, log_f)))
    return jnp.moveaxis(hs, 0, 2).reshape(bn, h, l, v.shape[-1]), state


def lru_run(a, b, h0):
    def combine(lhs, rhs):
        return (lhs[0] * rhs[0], rhs[0] * lhs[1] + rhs[1])
    a_cum, hs = lax.associative_scan(combine, (a, b), axis=1)
    hs = hs + a_cum * h0[:, None, :]
    return hs, hs[:, -1]


def two_segment(run, ctx_args, lat_args, state0, reverse, axis):
    flip = (lambda t: jnp.flip(t, axis)) if reverse else (lambda t: t)
    y_c, s_c = run(*[flip(t) for t in ctx_args], state0)
    y_l, _ = run(*[flip(t) for t in lat_args], s_c)
    return flip(y_c), flip(y_l)


def hgrn2_mixer(u_c, u_l, lb):
    def prep(u):
        q, i, g, z_f, z_b = jnp.split(u, 5, axis=-1)
        dirs = []
        for z, lbd in ((z_f, lb[0]), (z_b, lb[1])):
            f = lbd + (1.0 - lbd) * jax.nn.sigmoid(z)
            log_f = jnp.log(f)
            k = (1.0 - lbd) * jax.nn.sigmoid(-z)
            dirs.append((to_heads(k, HG_HEADS), to_heads(log_f, HG_HEADS)))
        return to_heads(q, HG_HEADS), to_heads(i, HG_HEADS), g, dirs

    qc, ic, gc, dc = prep(u_c)
    ql, il, gl, dl = prep(u_l)
    s0 = jnp.zeros((u_c.shape[0], HG_HEADS, HG_DK, HG_DK), jnp.float32)
    oc, ol = 0.0, 0.0
    for d, rev in enumerate((False, True)):
        yc, yl = two_segment(chunked_gla, (qc, dc[d][0], ic, dc[d][1]),
                             (ql, dl[d][0], il, dl[d][1]), s0, rev, 2)
        oc, ol = oc + yc, ol + yl

    def finish(o, g):
        o = head_rms(jnp.moveaxis(o, 1, 2))
        return o.reshape(g.shape) * jax.nn.silu(g)
    return finish(oc, gc), finish(ol, gl)


def ssd_mixer(u_c, u_l, conv_w, a_log, dt_bias, d_skip, norm_g):
    rep = SSD_HEADS // SSD_GROUPS

    def prep(u):
        z, xbc, dt = jnp.split(u, [SSD_W, SSD_W + SSD_CONV_DIM], axis=-1)
        xbc = jax.nn.silu(conv_centred(xbc, conv_w))
        xs, bm, cmat = jnp.split(xbc, [SSD_W, SSD_W + SSD_GROUPS * SSD_STATE], axis=-1)
        bh = jnp.repeat(to_heads(bm, SSD_GROUPS), rep, axis=1)
        ch = jnp.repeat(to_heads(cmat, SSD_GROUPS), rep, axis=1)
        dirs = []
        for d in range(2):
            delta = jax.nn.softplus(dt[..., d * SSD_HEADS:(d + 1) * SSD_HEADS] + dt_bias[d])
            delta = jnp.moveaxis(delta, 1, 2)[..., None]
            dirs.append((bh * delta, delta * (-jnp.exp(a_log[d]))[None, :, None, None]))
        return z, to_heads(xs, SSD_HEADS), ch, dirs

    zc, xc, cc, dc = prep(u_c)
    zl, xl, cl, dl = prep(u_l)
    s0 = jnp.zeros((u_c.shape[0], SSD_HEADS, SSD_STATE, SSD_HEADDIM), jnp.float32)
    yc = d_skip[None, :, None, None] * xc
    yl = d_skip[None, :, None, None] * xl
    for d, rev in enumerate((False, True)):
        oc, ol = two_segment(chunked_gla, (cc, dc[d][0], xc, dc[d][1]),
                             (cl, dl[d][0], xl, dl[d][1]), s0, rev, 2)
        yc, yl = yc + oc, yl + ol

    def finish(y, z):
        y = jnp.moveaxis(y, 1, 2).reshape(z.shape) * jax.nn.silu(z)
        y = head_rms(y.reshape(*z.shape[:-1], SSD_GROUPS, -1)).reshape(z.shape)
        return y * norm_g
    return finish(yc, zc), finish(yl, zl)


def blockdiag(x, w):
    bn, l, _ = x.shape
    return jnp.einsum('blni,nij->blnj', x.reshape(bn, l, LRU_BLOCKS, LRU_BW), w).reshape(bn, l, LRU_W)


def lru_mixer(u_c, u_l, conv_w, wa, ba, wx, bx, lam):
    bn, l, w = u_l.shape
    rows = l // GRID_W
    u_l = u_l.reshape(bn, rows, GRID_W, w).transpose(0, 2, 1, 3).reshape(bn, l, w)

    def prep(u):
        gate, xr = jnp.split(u, 2, axis=-1)
        xr = conv_centred(xr, conv_w)
        dirs = []
        for d in range(2):
            r = jax.nn.sigmoid(blockdiag(xr, wa[d]) + ba[d])
            i = jax.nn.sigmoid(blockdiag(xr, wx[d]) + bx[d])
            log_a = -LRU_C * jax.nn.softplus(-lam[d]) * r
            dirs.append((jnp.exp(log_a), jnp.sqrt(-jnp.expm1(2.0 * log_a)) * (i * xr)))
        return gate, dirs

    gc, dc = prep(u_c)
    gl, dl = prep(u_l)
    h0 = jnp.zeros((bn, LRU_W), jnp.float32)
    yc, yl = 0.0, 0.0
    for d, rev in enumerate((False, True)):
        oc, ol = two_segment(lru_run, dc[d], dl[d], h0, rev, 1)
        yc, yl = yc + oc, yl + ol
    yc = yc * jax.nn.gelu(gc)
    yl = yl * jax.nn.gelu(gl)
    yl = yl.reshape(bn, GRID_W, rows, LRU_W).transpose(0, 2, 1, 3).reshape(bn, l, LRU_W)
    return yc, yl


def mlstm_mixer(u_c, u_l, b_i, b_f):
    h = ML_HEADS

    def prep(u):
        q, k, v, o, ig, fg = jnp.split(u, [ML_W, 2 * ML_W, 3 * ML_W, 4 * ML_W, 4 * ML_W + 2 * h], axis=-1)
        dirs = []
        for d in range(2):
            li = jnp.moveaxis(ig[..., d * h:(d + 1) * h] + b_i[d], 1, 2)
            lf = jax.nn.log_sigmoid(jnp.moveaxis(fg[..., d * h:(d + 1) * h] + b_f[d], 1, 2))
            dirs.append((li, lf))
        return to_heads(q, h), to_heads(k, h) * ML_DH ** -0.5, to_heads(v, h), o, dirs

    qc, kc, vc, oc_g, dc = prep(u_c)
    ql, kl, vl, ol_g, dl = prep(u_l)
    bn = u_c.shape[0]
    s0 = (jnp.zeros((bn, h, ML_DH, ML_DH), jnp.float32), jnp.zeros((bn, h, ML_DH), jnp.float32),
          jnp.full((bn, h), STATE_NEG, jnp.float32))
    yc, yl = 0.0, 0.0
    for d, rev in enumerate((False, True)):
        oc, ol = two_segment(chunked_mlstm, (qc, kc, vc, *dc[d]), (ql, kl, vl, *dl[d]), s0, rev, 2)
        yc, yl = yc + oc, yl + ol

    def finish(y, o):
        return head_rms(jnp.moveaxis(y, 1, 2)).reshape(o.shape) * jax.nn.sigmoid(o)
    return finish(yc, oc_g), finish(yl, ol_g)


def token_mixers(h_c, h_l, w_in, w_out, lb, ssd_conv_w, ssd_a_log, ssd_dt_bias, ssd_d, ssd_norm_g,
                 lru_conv_w, lru_wa, lru_ba, lru_wx, lru_bx, lru_lambda, ml_b_i, ml_b_f):
    u_c = (h_c @ w_in).astype(jnp.float32)
    u_l = (h_l @ w_in).astype(jnp.float32)
    cuts = [HG_COLS, HG_COLS + SSD_COLS, HG_COLS + SSD_COLS + LRU_COLS]
    pc = jnp.split(u_c, cuts, axis=-1)
    pl = jnp.split(u_l, cuts, axis=-1)
    outs = [hgrn2_mixer(pc[0], pl[0], lb),
            ssd_mixer(pc[1], pl[1], ssd_conv_w, ssd_a_log, ssd_dt_bias, ssd_d, ssd_norm_g),
            lru_mixer(pc[2], pl[2], lru_conv_w, lru_wa, lru_ba, lru_wx, lru_bx, lru_lambda),
            mlstm_mixer(pc[3], pl[3], ml_b_i, ml_b_f)]
    y_c = jnp.concatenate([o[0] for o in outs], axis=-1)
    y_l = jnp.concatenate([o[1] for o in outs], axis=-1)
    return ((y_c.astype(w_out.dtype) @ w_out).astype(h_c.dtype),
            (y_l.astype(w_out.dtype) @ w_out).astype(h_l.dtype))


def hier_moe(t, w_gr, w_er, w_gate, w_up, w_down):
    n_tok, d = t.shape
    g_logits = (t @ w_gr).astype(jnp.float32)
    g_prob = jax.nn.softmax(g_logits, axis=-1)
    g_sel = jnp.argmax(g_logits, axis=-1)
    e_logits = (t @ w_er).astype(jnp.float32).reshape(n_tok, N_EXPERT_GROUPS, EXPERTS_PER_GROUP)
    within = jnp.take_along_axis(e_logits, g_sel[:, None, None], axis=1)[:, 0]
    top_v, top_i = lax.top_k(within, TOP_K)
    wts = jax.nn.softmax(top_v, axis=-1) * jnp.take_along_axis(g_prob, g_sel[:, None], axis=1)
    eid = g_sel[:, None] * EXPERTS_PER_GROUP + top_i

    n_assign = n_tok * TOP_K
    flat_e = eid.reshape(-1)
    order = jnp.argsort(flat_e)
    se = flat_e[order]
    tok = order // TOP_K
    counts = jnp.zeros((N_EXPERTS,), jnp.int32).at[flat_e].add(1)
    padded = (counts + MOE_BLOCK - 1) // MOE_BLOCK * MOE_BLOCK
    pend = jnp.cumsum(padded)
    pstart = pend - padded
    start = jnp.cumsum(counts) - counts
    dest = pstart[se] + jnp.arange(n_assign, dtype=jnp.int32) - start[se]
    n_blk = -(-n_assign // MOE_BLOCK) + N_EXPERTS
    slot_tok = jnp.zeros((n_blk * MOE_BLOCK,), jnp.int32).at[dest].set(tok.astype(jnp.int32))
    blk_start = jnp.arange(n_blk, dtype=jnp.int32) * MOE_BLOCK
    blk_exp = jnp.minimum(jnp.sum(blk_start[:, None] >= pend[None, :], axis=1), N_EXPERTS - 1)

    def run_block(args):
        toks, e = args
        xb = t[toks]
        hid = jax.nn.silu(xb @ w_gate[e]) * (xb @ w_up[e])
        return hid @ w_down[e]

    yb = lax.map(run_block, (slot_tok.reshape(n_blk, MOE_BLOCK), blk_exp)).reshape(-1, d)
    contrib = (yb[dest].astype(jnp.float32) * wts.reshape(-1)[order][:, None]).astype(t.dtype)
    return jnp.zeros_like(t).at[tok].add(contrib)


def setup_inputs(seed: int = 0) -> dict:
    key = jax.random.key(seed)
    ks = iter(jax.random.split(key, 40))
    f32 = jnp.float32
    d = D_MODEL

    def nrm(shape, scale):
        return jax.random.normal(next(ks), shape, f32) * scale

    def unif(shape, lo, hi):
        return jax.random.uniform(next(ks), shape, f32, minval=lo, maxval=hi)

    x = nrm((BATCH, SEQ, d), 1.0)
    c = nrm((BATCH, d), 1.0)
    ctx = nrm((BATCH, CTX_LEN, d), 1.0)
    c_ctx = nrm((d,), 1.0)
    ada_w = nrm((DEPTH, d, 6 * d), 0.5 * d ** -0.5)
    ada_b = nrm((DEPTH, 6 * d), 0.02)
    norm1_g = 1.0 + nrm((DEPTH, d), 0.05)
    norm2_g = 1.0 + nrm((DEPTH, d), 0.05)
    w_in = nrm((DEPTH, d, IN_COLS), d ** -0.5)
    w_out = nrm((DEPTH, MIX_W, d), MIX_W ** -0.5)
    hg_lb_logits = nrm((DEPTH, 2, HG_W), 0.1)
    ssd_conv_w = nrm((DEPTH, SSD_CONV, SSD_CONV_DIM), SSD_CONV ** -0.5)
    ssd_a_log = jnp.log(unif((DEPTH, 2, SSD_HEADS), 1.0, 16.0))
    dt0 = jnp.exp(unif((DEPTH, 2, SSD_HEADS), math.log(1e-3), math.log(1e-1)))
    ssd_dt_bias = dt0 + jnp.log(-jnp.expm1(-dt0))
    ssd_d = 1.0 + nrm((DEPTH, SSD_HEADS), 0.1)
    ssd_norm_g = 1.0 + nrm((DEPTH, SSD_W), 0.05)
    lru_conv_w = nrm((DEPTH, LRU_CONV, LRU_W), LRU_CONV ** -0.5)
    lru_wa = nrm((DEPTH, 2, LRU_BLOCKS, LRU_BW, LRU_BW), LRU_BW ** -0.5)
    lru_ba = nrm((DEPTH, 2, LRU_W), 0.02)
    lru_wx = nrm((DEPTH, 2, LRU_BLOCKS, LRU_BW, LRU_BW), LRU_BW ** -0.5)
    lru_bx = nrm((DEPTH, 2, LRU_W), 0.02)
    a0 = unif((DEPTH, 2, LRU_W), 0.9, 0.999)
    p = a0 ** (1.0 / LRU_C)
    lru_lambda = jnp.log(p) - jnp.log1p(-p)
    ml_b_i = nrm((DEPTH, 2, ML_HEADS), 0.1)
    ml_b_f = unif((DEPTH, 2, ML_HEADS), 3.0, 6.0)
    router_group_w = nrm((DEPTH, d, N_EXPERT_GROUPS), d ** -0.5)
    router_expert_w = nrm((DEPTH, d, N_EXPERTS), d ** -0.5)
    moe_w_gate = nrm((DEPTH, N_EXPERTS, d, D_EXPERT), d ** -0.5)
    moe_w_up = nrm((DEPTH, N_EXPERTS, d, D_EXPERT), d ** -0.5)
    moe_w_down = nrm((DEPTH, N_EXPERTS, D_EXPERT, d), D_EXPERT ** -0.5)
    final_norm_g = 1.0 + nrm((d,), 0.05)
    return {'x': x, 'c': c, 'ctx': ctx, 'c_ctx': c_ctx, 'ada_w': ada_w, 'ada_b': ada_b,
            'norm1_g': norm1_g, 'norm2_g': norm2_g, 'w_in': w_in, 'w_out': w_out,
            'hg_lb_logits': hg_lb_logits, 'ssd_conv_w': ssd_conv_w, 'ssd_a_log': ssd_a_log,
            'ssd_dt_bias': ssd_dt_bias, 'ssd_d': ssd_d, 'ssd_norm_g': ssd_norm_g,
            'lru_conv_w': lru_conv_w, 'lru_wa': lru_wa, 'lru_ba': lru_ba, 'lru_wx': lru_wx,
            'lru_bx': lru_bx, 'lru_lambda': lru_lambda, 'ml_b_i': ml_b_i, 'ml_b_f': ml_b_f,
            'router_group_w': router_group_w, 'router_expert_w': router_expert_w,
            'moe_w_gate': moe_w_gate, 'moe_w_up': moe_w_up, 'moe_w_down': moe_w_down,
            'final_norm_g': final_norm_g}


def reference(x, c, ctx, c_ctx, ada_w, ada_b, norm1_g, norm2_g, w_in, w_out, hg_lb_logits,
              ssd_conv_w, ssd_a_log, ssd_dt_bias, ssd_d, ssd_norm_g, lru_conv_w, lru_wa, lru_ba,
              lru_wx, lru_bx, lru_lambda, ml_b_i, ml_b_f, router_group_w, router_expert_w,
              moe_w_gate, moe_w_up, moe_w_down, final_norm_g):
    d = x.shape[-1]
    p = jax.nn.softmax(hg_lb_logits.astype(jnp.float32), axis=0)
    lb_all = jnp.cumsum(p, axis=0) - p[:1]
    s_lat = jax.nn.silu(c)
    s_ctx = jax.nn.silu(c_ctx)
    xl, xc = x, ctx
    for l in range(DEPTH):
        mod_l = jnp.split((s_lat @ ada_w[l] + ada_b[l])[:, None, :], 6, axis=-1)
        mod_c = jnp.split(s_ctx @ ada_w[l] + ada_b[l], 6, axis=-1)
        hl = rmsnorm(xl, norm1_g[l]) * (1.0 + mod_l[1]) + mod_l[0]
        hc = rmsnorm(xc, norm1_g[l]) * (1.0 + mod_c[1]) + mod_c[0]
        yc, yl = token_mixers(hc, hl, w_in[l], w_out[l], lb_all[l], ssd_conv_w[l], ssd_a_log[l],
                              ssd_dt_bias[l], ssd_d[l], ssd_norm_g[l], lru_conv_w[l], lru_wa[l],
                              lru_ba[l], lru_wx[l], lru_bx[l], lru_lambda[l], ml_b_i[l], ml_b_f[l])
        xl = xl + mod_l[2] * yl
        hl = rmsnorm(xl, norm2_g[l]) * (1.0 + mod_l[4]) + mod_l[3]
        moe_args = (router_group_w[l], router_expert_w[l], moe_w_gate[l], moe_w_up[l], moe_w_down[l])
        if l < DEPTH - 1:
            xc = xc + mod_c[2] * yc
            hc = rmsnorm(xc, norm2_g[l]) * (1.0 + mod_c[4]) + mod_c[3]
            n_ctx = xc.shape[0] * xc.shape[1]
            y = hier_moe(jnp.concatenate([hc.reshape(-1, d), hl.reshape(-1, d)], axis=0), *moe_args)
            xc = xc + mod_c[5] * y[:n_ctx].reshape(xc.shape)
            xl = xl + mod_l[5] * y[n_ctx:].reshape(xl.shape)
        else:
            xl = xl + mod_l[5] * hier_moe(hl.reshape(-1, d), *moe_args).reshape(xl.shape)
    return rmsnorm(xl, final_norm_g)
```

```python
import contextlib
import numpy as np
import concourse.bass as bass
import concourse.mybir as mybir
from concourse.bass_utils import run_bass_kernel_spmd

F32 = mybir.dt.float32
BF16 = mybir.dt.bfloat16
AF = mybir.ActivationFunctionType
ALU = mybir.AluOpType
AX = mybir.AxisListType

D = 2048
KC = 16
IN_COLS = 7200
NORM_EPS = 1e-6


class Buf:
    def __init__(self, t, name):
        self.t = t
        self.name = name
        self.w = None
        self.r = []

    def __getitem__(self, idx):
        return View(self, self.t[idx])

    @property
    def v(self):
        return View(self, self.t[:])


class View:
    def __init__(self, buf, ap):
        self.buf = buf
        self.ap = ap

    def __getitem__(self, idx):
        return View(self.buf, self.ap[idx])

    def bitcast(self, dt):
        return View(self.buf, self.ap.bitcast(dt))

    def bc(self, shape):
        return View(self.buf, self.ap.to_broadcast(shape))

    def rr(self, pat, **kw):
        return View(self.buf, self.ap.rearrange(pat, **kw))


def _ap(x):
    return x.ap if isinstance(x, View) else x


class Sched:
    NDMA = 8

    def __init__(self, nc, es):
        self.nc = nc
        self.es = es
        self.eng = {'pe': nc.tensor, 'dve': nc.vector, 'act': nc.scalar,
                    'pool': nc.gpsimd, 'sp': nc.sync}
        self.sem = {}
        self.cnt = {}
        for k in self.eng:
            self.sem[k] = es.enter_context(nc.semaphore('s_' + k))
            self.cnt[k] = 0
        self.dsem = {}
        self.dcnt = {}
        self.dnext = {}
        for q in ('sp', 'pool'):
            for i in range(self.NDMA):
                self.dsem[(q, i)] = es.enter_context(nc.semaphore('d_%s%d' % (q, i)))
                self.dcnt[(q, i)] = 0
            self.dnext[q] = 0
        self.seen = {k: {} for k in self.eng}
        self.nbuf = 0
        self.ninstr = 0

    def sb(self, shape, dt=F32, name=None, es=None):
        self.nbuf += 1
        name = (name or 'b') + '_%d' % self.nbuf
        t = (es or self.es).enter_context(self.nc.sbuf_tensor(name, list(shape), dt))
        return Buf(t, name)

    def ps(self, shape, dt=F32, name=None, es=None):
        self.nbuf += 1
        name = (name or 'p') + '_%d' % self.nbuf
        t = (es or self.es).enter_context(self.nc.psum_tensor(name, list(shape), dt))
        return Buf(t, name)

    def dram(self, shape, dt=F32, name=None):
        self.nbuf += 1
        name = (name or 'scr') + '_%d' % self.nbuf
        t = self.nc.dram_tensor(name, list(shape), dt, kind="Internal").ap()
        return Buf(t, name)

    def _semh(self, key):
        return self.sem[key] if key in self.sem else self.dsem[key]

    def _wait(self, e, tok):
        if tok is None:
            return
        key, val = tok
        if self.seen[e].get(key, 0) >= val:
            return
        self.eng[e].wait_ge(self._semh(key), val)
        self.seen[e][key] = val

    def _need(self, e, w, r):
        toks = []
        for v in r:
            if isinstance(v, View) and v.buf.w is not None:
                toks.append(v.buf.w)
        for v in w:
            if isinstance(v, View):
                b = v.buf
                if b.w is not None and (b.w[0] != e or e != 'pe'):
                    toks.append(b.w)
                for t in b.r:
                    if t[0] != e or e != 'pe':
                        toks.append(t)
        best = {}
        for key, val in toks:
            if self.seen[e].get(key, 0) >= val:
                continue
            if best.get(key, 0) < val:
                best[key] = val
        return list(best.items())

    def _deps(self, e, w, r):
        toks = []
        for v in r:
            if isinstance(v, View) and v.buf.w is not None:
                toks.append(v.buf.w)
        for v in w:
            if isinstance(v, View):
                b = v.buf
                if b.w is not None and (b.w[0] != e or e != 'pe'):
                    toks.append(b.w)
                for t in b.r:
                    if t[0] != e or e != 'pe':
                        toks.append(t)
        for t in toks:
            self._wait(e, t)

    def _commit(self, tok, w, r):
        for v in r:
            if isinstance(v, View):
                v.buf.r.append(tok)
        for v in w:
            if isinstance(v, View):
                v.buf.w = tok
                v.buf.r = []

    def op(self, e, fn, w=(), r=(), accum=False, attach=None):
        if attach is None:
            attach = e in ('dve', 'act', 'pool')
        need = self._need(e, w, r)
        last = need.pop() if (attach and need) else None
        for t in need:
            self._wait(e, t)
        ins = fn(self.eng[e])
        if last is not None:
            ins._wait_ge(self._semh(last[0]), last[1])
            self.seen[e][last[0]] = last[1]
        self.cnt[e] += 1
        ins.then_inc(self.sem[e], 1)
        tok = (e, self.cnt[e])
        self.ninstr += 1
        if accum:
            for v in r:
                if isinstance(v, View):
                    v.buf.r.append(tok)
            for v in w:
                v.buf.w = tok
        else:
            self._commit(tok, w, r)
        return tok

    def dma(self, q, out, in_, **kw):
        w = [out] if isinstance(out, View) else []
        r = [in_] if isinstance(in_, View) else []
        i = self.dnext[q]
        self.dnext[q] = (i + 1) % self.NDMA
        key = (q, i)
        if self.dcnt[key] > 0:
            self._wait(q, (key, self.dcnt[key]))
        self._deps(q, w, r)
        ins = self.eng[q].dma_start(out=_ap(out), in_=_ap(in_), **kw)
        self.dcnt[key] += 16
        ins.then_inc(self.dsem[key], 16)
        tok = (key, self.dcnt[key])
        self.ninstr += 1
        self._commit(tok, w, r)
        return tok

    def barrier(self):
        for e in self.eng:
            for k in ('pe', 'dve', 'act', 'pool'):
                if k != e and self.cnt[k]:
                    self._wait(e, (k, self.cnt[k]))
            for key, c in self.dcnt.items():
                if c:
                    self._wait(e, (key, c))

    def finish(self):
        for k in ('pe', 'dve', 'act', 'pool'):
            if self.cnt[k]:
                self._wait('sp', (k, self.cnt[k]))
        for key, c in self.dcnt.items():
            if c:
                self._wait('sp', (key, c))

    def mm(self, out, lhsT, rhs, start=True, stop=True):
        lw = lhsT.buf.w if isinstance(lhsT, View) else None
        safe = lw is None or self.seen['pe'].get(lw[0], 0) >= lw[1]
        return self.op('pe', lambda e: e.matmul(_ap(out), _ap(lhsT), _ap(rhs), start=start, stop=stop),
                       w=[out], r=[lhsT, rhs], accum=not start, attach=safe)

    def tr(self, out, in_, ident):
        return self.op('pe', lambda e: e.transpose(_ap(out), _ap(in_), _ap(ident)), w=[out], r=[in_, ident])

    def act(self, out, in_, func, bias=None, scale=1.0):
        r = [in_] + [x for x in (bias, scale) if isinstance(x, View)]
        kw = {}
        if bias is not None:
            kw['bias'] = _ap(bias)
        return self.op('act', lambda e: e.activation(out=_ap(out), in_=_ap(in_), func=func,
                                                     scale=_ap(scale), **kw), w=[out], r=r)

    def tt(self, out, a, b, op, eng='dve'):
        return self.op(eng, lambda e: e.tensor_tensor(out=_ap(out), in0=_ap(a), in1=_ap(b), op=op),
                       w=[out], r=[a, b])

    def ts(self, out, a, s1, op0, s2=None, op1=None, eng='dve'):
        r = [a] + [s for s in (s1, s2) if isinstance(s, View)]
        if op1 is None:
            return self.op(eng, lambda e: e.tensor_scalar(out=_ap(out), in0=_ap(a), scalar1=_ap(s1),
                                                          scalar2=None, op0=op0), w=[out], r=r)
        return self.op(eng, lambda e: e.tensor_scalar(out=_ap(out), in0=_ap(a), scalar1=_ap(s1),
                                                      scalar2=_ap(s2), op0=op0, op1=op1), w=[out], r=r)

    def stt(self, out, a, s, b, op0, op1):
        r = [a, b] + ([s] if isinstance(s, View) else [])
        return self.op('dve', lambda e: e.scalar_tensor_tensor(out=_ap(out), in0=_ap(a), scalar=_ap(s),
                                                               in1=_ap(b), op0=op0, op1=op1), w=[out], r=r)

    def copy(self, out, in_, eng='dve'):
        if eng == 'act':
            return self.op('act', lambda e: e.copy(out=_ap(out), in_=_ap(in_)), w=[out], r=[in_])
        return self.op(eng, lambda e: e.tensor_copy(out=_ap(out), in_=_ap(in_)), w=[out], r=[in_])

    def memset(self, out, val, eng='pool'):
        return self.op(eng, lambda e: e.memset(_ap(out), val), w=[out], r=[])

    def red(self, out, in_, op, eng='dve'):
        return self.op(eng, lambda e: e.tensor_reduce(out=_ap(out), in_=_ap(in_), op=op, axis=AX.X),
                       w=[out], r=[in_])

    def recip(self, out, in_):
        return self.op('dve', lambda e: e.reciprocal(out=_ap(out), in_=_ap(in_)), w=[out], r=[in_])

    def scan(self, out, d0, d1, init):
        r = [d0, d1] + ([init] if isinstance(init, View) else [])
        return self.op('dve', lambda e: e.tensor_tensor_scan(out=_ap(out), data0=_ap(d0), data1=_ap(d1),
                                                             initial=_ap(init), op0=ALU.mult, op1=ALU.add),
                       w=[out], r=r)


def gla_vloop(S, items, n, rev, identb, vvb, qq, vps, tm, st, pp):
    NB = len(vps)

    def bc(i):
        row = items[i][0]
        if row is not None:
            S.mm(vps[i % NB][:, :n], identb[:, row:row + 1].bc([128, 128]), vvb[:, :n])

    for i in range(min(2, len(items))):
        bc(i)
    for i, (row, kk, dec, stt, readout) in enumerate(items):
        b = i % NB
        if row is not None:
            S.tt(tm[b][:, :n], vps[b][:, :n], kk[:, :n], ALU.mult)
            src_ = tm[b]
        else:
            src_ = kk
        if not rev:
            S.scan(st[b][:, :n], dec[:, :n], src_[:, :n], stt)
            S.copy(stt, st[b][:, n - 1:n], 'act')
        else:
            S.scan(st[b][:, :n][:, ::-1], dec[:, :n][:, ::-1], src_[:, :n][:, ::-1], stt)
            S.copy(stt, st[b][:, 0:1], 'act')
        S.tt(pp[b][:, :n], st[b][:, :n], qq[:, :n], ALU.mult, eng='pool')
        if i + 2 < len(items):
            bc(i + 2)
        readout(pp[b][:, :n])


def blocks_of(ctx, seq, nt=512):
    out = []
    for s in range(0, ctx, nt):
        out.append((s, min(nt, ctx - s)))
    for s in range(0, seq, nt):
        out.append((ctx + s, min(nt, seq - s)))
    return out


def build(depth, ctx, seq, stage='full'):
    T = ctx + seq
    nc = bass.Bass("TRN2", target_bir_lowering=False)

    def din(name, shape):
        return nc.dram_tensor(name, list(shape), F32, kind="ExternalInput").ap()

    xT_in = din("xT", [D, T])
    sv_in = din("sv", [128, KC, 2])
    ada_w = din("ada_w", [depth, D, 6 * D])
    ada_b = din("ada_b", [depth, 128, 96])
    n1g = din("n1g", [depth, 128, KC])
    n2g = din("n2g", [depth, 128, KC])
    fng = din("fng", [128, KC])
    w_in = din("w_in", [depth, D, IN_COLS])
    ident_in = din("ident", [128, 128])
    lbl_in = din("lbl", [128, 8, depth])
    mlb_in = din("mlb", [16, depth])
    scw_in = din("scw", [128, depth, 8, 4])
    alog_in = din("alog", [128, depth, 16])
    dtb_in = din("dtb", [16, depth])
    dsk_in = din("dsk", [128, depth, 4])
    sng_in = din("sng", [128, depth, 4])
    lcw_in = din("lcw", [128, depth, 4, 4])
    lam_in = din("lam", [128, depth, 2, 4])
    lba_in = din("lba", [128, depth, 2, 4])
    lbx_in = din("lbx", [128, depth, 2, 4])
    lwa_in = din("lwa", [depth, 2, 4, 128, 128])
    lwx_in = din("lwx", [depth, 2, 4, 128, 128])
    w_out_in = din("w_out", [depth, D, D])
    out_xm = None
    if stage == 'mix':
        out_xm = nc.dram_tensor("xmT", [D, T], F32, kind="ExternalOutput").ap()
    rw_in = din("rw", [depth, 128, KC, 36])
    wg_in = din("moe_w_gate", [depth, 32, D, 512])
    wu_in = din("moe_w_up", [depth, 32, D, 512])
    wd_in = din("moe_w_down", [depth, 32, 512, D])
    out_lr = None
    if stage == 'lr':
        out_lr = nc.dram_tensor("lrT", [512, T], F32, kind="ExternalOutput").ap()
    out_ss = None
    if stage == 'ss':
        out_ss = nc.dram_tensor("ssT", [512, T], F32, kind="ExternalOutput").ap()
    out_ml = None
    if stage == 'ml':
        out_ml = nc.dram_tensor("mlT", [512, T], F32, kind="ExternalOutput").ap()
    out_hg = None
    if stage == 'hg':
        out_hg = nc.dram_tensor("hgT", [512, T], F32, kind="ExternalOutput").ap()
    out_u = None
    if stage == 'u':
        out_u = nc.dram_tensor("uT", [IN_COLS, T], F32, kind="ExternalOutput").ap()
    outT = nc.dram_tensor("outT", [D, seq], F32, kind="ExternalOutput").ap()

    blks = blocks_of(ctx, seq)
    with contextlib.ExitStack() as es:
        S = Sched(nc, es)
        ones = S.sb([128, 128], F32, 'ones')
        S.memset(ones.v, 1.0)
        ident = S.sb([128, 128], F32, 'ident')
        S.dma('sp', ident.v, ident_in)
        zsel = S.sb([128, 255], F32, 'zsel')
        S.memset(zsel.v, 0.0)
        S.memset(zsel[:, 127:128], 1.0)
        identb = S.sb([128, 128], BF16, 'identb')
        S.copy(identb.v, ident.v)
        zselb = S.sb([128, 255], BF16, 'zselb')
        S.copy(zselb.v, zsel.v)
        onesb = S.sb([128, 128], BF16, 'onesb')
        S.copy(onesb.v, ones.v)
        lbl = S.sb([128, 8, depth], F32, 'lbl')
        S.dma('sp', lbl.v, lbl_in)
        lmx = S.sb([128, 8], F32, 'lmx')
        S.red(lmx.v, lbl.v, ALU.max)
        lbe = S.sb([128, 8, depth], F32, 'lbe')
        S.tt(lbe.v, lbl.v, lmx.v.rr("p (a o) -> p a o", o=1).bc([128, 8, depth]), ALU.subtract)
        S.act(lbe.v, lbe.v, AF.Exp)
        lsm = S.sb([128, 8], F32, 'lsm')
        S.red(lsm.v, lbe.v, ALU.add)
        S.recip(lsm.v, lsm.v)
        S.tt(lbe.v, lbe.v, lsm.v.rr("p (a o) -> p a o", o=1).bc([128, 8, depth]), ALU.mult)
        LB = S.sb([128, 8, depth], F32, 'LB')
        OMLB = S.sb([128, 8, depth], F32, 'OMLB')
        S.memset(LB.v, 0.0)
        for l_ in range(1, depth):
            S.tt(LB[:, :, l_], LB[:, :, l_ - 1], lbe[:, :, l_], ALU.add)
        S.ts(OMLB.v, LB.v, -1.0, ALU.mult, 1.0, ALU.add)
        X = [S.dram([D, n], F32, 'X') for (_, n) in blks]
        for bi, (t0, n) in enumerate(blks):
            S.dma('sp', X[bi].v, xT_in[:, t0:t0 + n])

        nb = len(blks)
        HQ = [[S.dram([128, n], F32, 'HQ') for (_, n) in blks] for _ in range(4)]
        HV = [[S.dram([128, n], F32, 'HV') for (_, n) in blks] for _ in range(4)]
        HG = [[S.dram([128, n], F32, 'HG') for (_, n) in blks] for _ in range(4)]
        HF = [[[S.dram([128, n], F32, 'HF') for (_, n) in blks] for _ in range(4)] for _ in range(2)]
        OF = [S.dram([128, n], F32, 'OF') for (_, n) in blks]
        YS = [[S.dram([128, n], F32, 'YS') for (_, n) in blks] for _ in range(16)]
        MQ = [[S.dram([128, n], F32, 'MQ') for (_, n) in blks] for _ in range(4)]
        MK = [[S.dram([128, n], F32, 'MK') for (_, n) in blks] for _ in range(4)]
        MV = [[S.dram([128, n], F32, 'MV') for (_, n) in blks] for _ in range(4)]
        MO = [[S.dram([128, n], F32, 'MO') for (_, n) in blks] for _ in range(4)]
        MGE = [S.dram([16, n], F32, 'MGE') for (_, n) in blks]
        MGS = [S.dram([16, n], F32, 'MGS') for (_, n) in blks]
        SZ = [[S.dram([128, n], F32, 'SZ') for (_, n) in blks] for _ in range(4)]
        SXR = [[S.dram([128, n], F32, 'SXR') for (_, n) in blks] for _ in range(8)]
        SXC = [[S.dram([128, n], F32, 'SXC') for (_, n) in blks] for _ in range(8)]
        SD = [S.dram([16, n], F32, 'SD') for (_, n) in blks]
        SY = [[S.dram([128, n], F32, 'SY') for (_, n) in blks] for _ in range(4)]
        scw = S.sb([128, depth, 8, 4], F32, 'scw')
        S.dma('sp', scw.v, scw_in)
        nega = S.sb([128, depth, 16], F32, 'nega')
        S.dma('sp', nega.v, alog_in)
        S.act(nega.v, nega.v, AF.Exp)
        S.ts(nega.v, nega.v, -1.0, ALU.mult)
        dtb = S.sb([16, depth], F32, 'dtb')
        S.dma('sp', dtb.v, dtb_in)
        dsk = S.sb([128, depth, 4], F32, 'dsk')
        S.dma('sp', dsk.v, dsk_in)
        sng = S.sb([128, depth, 4], F32, 'sng')
        S.dma('sp', sng.v, sng_in)
        LG = [[S.dram([128, n], F32, 'LG') for (_, n) in blks] for _ in range(4)]
        LXR = [[S.dram([128, n], F32, 'LXR') for (_, n) in blks] for _ in range(4)]
        lcw = S.sb([128, depth, 4, 4], F32, 'lcw')
        S.dma('sp', lcw.v, lcw_in)
        lba = S.sb([128, depth, 2, 4], F32, 'lba')
        S.dma('sp', lba.v, lba_in)
        lbx = S.sb([128, depth, 2, 4], F32, 'lbx')
        S.dma('sp', lbx.v, lbx_in)
        lam = S.sb([128, depth, 2, 4], F32, 'lam')
        S.dma('sp', lam.v, lam_in)
        CL = S.sb([128, depth, 2, 4], F32, 'CL')
        lt = S.sb([128, depth, 2, 4], F32, 'lt')
        S.act(lt.v, lam.v, AF.Abs)
        S.act(lt.v, lt.v, AF.Exp, scale=-1.0)
        S.ts(lt.v, lt.v, 1.0, ALU.add)
        S.act(lt.v, lt.v, AF.Ln)
        S.ts(CL.v, lam.v, -1.0, ALU.mult, 0.0, ALU.max)
        S.tt(CL.v, CL.v, lt.v, ALU.add)
        S.ts(CL.v, CL.v, -8.0, ALU.mult)
        mlb = S.sb([16, depth], F32, 'mlb')
        S.dma('sp', mlb.v, mlb_in)
        CHUNKS = []
        for r_ in range(5):
            for h_ in range(4):
                CHUNKS.append(((r_ * 4 + h_) * 128, 128, r_, h_))
        for j_ in range(4):
            CHUNKS.append((2560 + j_ * 128, 128, 'sz', j_))
        for j_ in range(8):
            CHUNKS.append((3072 + j_ * 128, 128, 'sx', j_))
        CHUNKS.append((4096, 16, 'sdt', 0))
        for j_ in range(4):
            CHUNKS.append((4112 + j_ * 128, 128, 'lg', j_))
        for j_ in range(4):
            CHUNKS.append((4624 + j_ * 128, 128, 'lx', j_))
        for ri_, rn_ in enumerate(('mq', 'mk', 'mv', 'mo')):
            for j_ in range(4):
                CHUNKS.append((5136 + ri_ * 512 + j_ * 128, 128, rn_, j_))
        CHUNKS.append((7184, 16, 'mg', 0))
        ctx_b = [i for i, (t0, _) in enumerate(blks) if t0 < ctx]
        lat_b = [i for i, (t0, _) in enumerate(blks) if t0 >= ctx]
        order_f = ctx_b + lat_b
        order_b = ctx_b[::-1] + lat_b[::-1]
        sv = S.sb([128, KC, 2], F32, 'sv')
        S.dma('sp', sv.v, sv_in)
        ssv = S.sb([128, KC, 2], F32, 'ssv')
        S.act(ssv.v, sv.v, AF.Silu)

        for l in range(depth):
            with contextlib.ExitStack() as ph:
                MOD = S.sb([128, 96, 2], F32, 'MOD')
                adb = S.sb([128, 96], F32, 'adb', es=ph)
                S.dma('sp', adb.v, ada_b[l])
                wst = [S.sb([128, KC, 512], F32, 'awst', es=ph) for _ in range(2)]
                mps = [S.ps([128, 512], F32, 'mps', es=ph) for _ in range(2)]
                for g in range(24):
                    wt = wst[g % 2]
                    for q4 in range(4):
                        S.dma('sp' if q4 % 2 == 0 else 'pool', wt[:, q4 * 4:(q4 + 1) * 4, :],
                              ada_w[l, q4 * 512:(q4 + 1) * 512, g * 512:(g + 1) * 512]
                              .rearrange("(k p) c -> p k c", p=128))
                    for j in range(4):
                        fc = g * 4 + j
                        p = mps[fc % 2]
                        for k in range(KC):
                            S.mm(p[:, 0:2], wt[:, k, j * 128:(j + 1) * 128], ssv[:, k, :],
                                 start=(k == 0), stop=(k == KC - 1))
                        S.ts(MOD[:, fc, :], p[:, 0:2], adb[:, fc:fc + 1], ALU.add)
                S.barrier()
            g1 = S.sb([128, KC], F32, 'g1')
            g2 = S.sb([128, KC], F32, 'g2')
            S.dma('sp', g1.v, n1g[l])
            S.dma('sp', g2.v, n2g[l])
            A1 = S.sb([128, KC, 2], F32, 'A1')
            A2 = S.sb([128, KC, 2], F32, 'A2')
            for j in range(2):
                S.stt(A1[:, :, j], MOD[:, 16:32, j], 1.0, g1.v, ALU.add, ALU.mult)
                S.stt(A2[:, :, j], MOD[:, 64:80, j], 1.0, g2.v, ALU.add, ALU.mult)

            with contextlib.ExitStack() as ph:
                NT = 512
                xb = [S.sb([128, KC, NT], F32, 'xb', es=ph) for _ in range(2)]
                sq = S.sb([128, KC, NT], F32, 'sq', es=ph)
                hT = S.sb([128, KC, NT], BF16, 'hT', es=ph)
                rstd = S.sb([128, NT], F32, 'rstd', es=ph)
                tmp = S.sb([128, NT], F32, 'tmpn', es=ph)
                ssp = S.ps([128, NT], F32, 'ssp', es=ph)
                wstg = [S.sb([128, KC, 128], F32, 'wstg', es=ph) for _ in range(2)]
                wbf = [S.sb([128, KC, 128], BF16, 'wbf', es=ph) for _ in range(2)]
                ups = [S.ps([128, NT], F32, 'ups', es=ph) for _ in range(2)]
                uo = [S.sb([128, NT], F32, 'uo', es=ph) for _ in range(2)]
                uo2 = S.sb([128, NT], F32, 'uo2', es=ph)
                for bi, (t0, n) in enumerate(blks):
                    j = 1 if t0 < ctx else 0
                    x_ = xb[bi % 2]
                    for q4 in range(4):
                        S.dma('sp' if q4 % 2 == 0 else 'pool', x_[:, q4 * 4:(q4 + 1) * 4, :n],
                              X[bi][q4 * 512:(q4 + 1) * 512, :].rr("(k p) t -> p k t", p=128))
                    S.act(sq[:, :, :n], x_[:, :, :n], AF.Square)
                    for k in range(KC):
                        S.mm(ssp[:, :n], ones.v, sq[:, k, :n], start=(k == 0), stop=(k == KC - 1))
                    S.ts(tmp[:, :n], ssp[:, :n], 1.0 / D, ALU.mult, NORM_EPS, ALU.add)
                    S.act(tmp[:, :n], tmp[:, :n], AF.Sqrt)
                    S.recip(rstd[:, :n], tmp[:, :n])
                    for k in range(KC):
                        S.tt(sq[:, k, :n], x_[:, k, :n], rstd[:, :n], ALU.mult)
                        S.ts(hT[:, k, :n], sq[:, k, :n], A1[:, k, j:j + 1], ALU.mult,
                             MOD[:, k, j:j + 1], ALU.add)
                    for c, (c0, ncol, role, hh) in enumerate(CHUNKS):
                        ws_, wb_ = wstg[c % 2], wbf[c % 2]
                        for q2 in range(2):
                            S.dma('sp' if q2 == 0 else 'pool', ws_[:, q2 * 8:(q2 + 1) * 8, :ncol],
                                  w_in[l, q2 * 1024:(q2 + 1) * 1024, c0:c0 + ncol]
                                  .rearrange("(k p) c -> p k c", p=128))
                        S.copy(wb_[:, :, :ncol], ws_[:, :, :ncol], 'act' if c % 2 else 'dve')
                        p = ups[c % 2]
                        for k in range(KC):
                            S.mm(p[:ncol, :n], wb_[:, k, :ncol], hT[:, k, :n],
                                 start=(k == 0), stop=(k == KC - 1))
                        o_ = uo[c % 2]
                        if out_u is not None:
                            S.copy(o_[:ncol, :n], p[:ncol, :n], 'dve' if c % 2 else 'act')
                            S.dma('sp', out_u[c0:c0 + ncol, t0:t0 + n], o_[:ncol, :n])
                        elif role == 'sz':
                            S.act(o_[:, :n], p[:, :n], AF.Silu)
                            S.dma('sp', SZ[hh][bi].v, o_[:, :n])
                        elif role == 'sx':
                            S.copy(o_[:, :n], p[:, :n], 'act')
                            S.dma('sp', SXR[hh][bi].v, o_[:, :n])
                        elif role == 'sdt':
                            o2 = uo2
                            S.act(o2[:16, :n], p[:16, :n], AF.Abs, bias=dtb[:, l:l + 1])
                            S.act(o2[:16, :n], o2[:16, :n], AF.Exp, scale=-1.0)
                            S.ts(o2[:16, :n], o2[:16, :n], 1.0, ALU.add)
                            S.act(o2[:16, :n], o2[:16, :n], AF.Ln)
                            S.ts(o_[:16, :n], p[:16, :n], dtb[:, l:l + 1], ALU.add)
                            S.ts(o_[:16, :n], o_[:16, :n], 0.0, ALU.max)
                            S.tt(o_[:16, :n], o_[:16, :n], o2[:16, :n], ALU.add)
                            S.dma('sp', SD[bi].v, o_[:16, :n])
                        elif role == 'lx':
                            S.copy(o_[:, :n], p[:, :n], 'act')
                            S.dma('sp', LXR[hh][bi].v, o_[:, :n])
                        elif role == 'lg':
                            o2 = uo2
                            S.act(o2[:, :n], p[:, :n], AF.Square)
                            S.ts(o2[:, :n], o2[:, :n], 0.044715, ALU.mult, 1.0, ALU.add)
                            S.tt(o2[:, :n], o2[:, :n], p[:, :n], ALU.mult)
                            S.act(o2[:, :n], o2[:, :n], AF.Sigmoid, scale=1.5957691216057308)
                            S.tt(o_[:, :n], o2[:, :n], p[:, :n], ALU.mult)
                            S.dma('sp', LG[hh][bi].v, o_[:, :n])
                        elif role in ('mq', 'mv'):
                            S.copy(o_[:, :n], p[:, :n], 'act')
                            S.dma('sp', (MQ if role == 'mq' else MV)[hh][bi].v, o_[:, :n])
                        elif role == 'mk':
                            S.ts(o_[:, :n], p[:, :n], 128.0 ** -0.5, ALU.mult)
                            S.dma('sp', MK[hh][bi].v, o_[:, :n])
                        elif role == 'mo':
                            S.act(o_[:, :n], p[:, :n], AF.Sigmoid)
                            S.dma('sp', MO[hh][bi].v, o_[:, :n])
                        elif role == 'mg':
                            S.act(o_[:16, :n], p[:16, :n], AF.Exp, bias=mlb[:, l:l + 1])
                            S.dma('sp', MGE[bi].v, o_[:16, :n])
                            o2 = uo2
                            S.act(o2[:16, :n], p[:16, :n], AF.Sigmoid, bias=mlb[:, l:l + 1])
                            S.dma('sp', MGS[bi].v, o2[:16, :n])
                        elif isinstance(role, int):
                            if role == 0:
                                S.copy(o_[:, :n], p[:, :n], 'act')
                                S.dma('sp', HQ[hh][bi].v, o_[:, :n])
                            elif role == 1:
                                S.copy(o_[:, :n], p[:, :n], 'act')
                                S.dma('sp', HV[hh][bi].v, o_[:, :n])
                            elif role == 2:
                                S.act(o_[:, :n], p[:, :n], AF.Silu)
                                S.dma('sp', HG[hh][bi].v, o_[:, :n])
                            else:
                                dd = role - 3
                                S.act(o_[:, :n], p[:, :n], AF.Sigmoid)
                                S.ts(o_[:, :n], o_[:, :n], OMLB[:, dd * 4 + hh, l:l + 1], ALU.mult,
                                     LB[:, dd * 4 + hh, l:l + 1], ALU.add)
                                S.dma('sp', HF[dd][hh][bi].v, o_[:, :n])
                S.barrier()
            if stage == 'u':
                break

            with contextlib.ExitStack() as ph:
                NT = 512
                dec = [S.sb([128, NT], F32, 'dec', es=ph) for _ in range(2)]
                kk = [S.sb([128, NT], F32, 'kk', es=ph) for _ in range(2)]
                qq = [S.sb([128, NT], F32, 'qq', es=ph) for _ in range(2)]
                vv = [S.sb([128, NT], F32, 'vv', es=ph) for _ in range(2)]
                tm = [S.sb([128, NT], F32, 'tm', es=ph) for _ in range(4)]
                st = [S.sb([128, NT], F32, 'st', es=ph) for _ in range(4)]
                pp = [S.sb([128, NT], BF16, 'pp', es=ph) for _ in range(4)]
                vvb = [S.sb([128, NT], BF16, 'vvb', es=ph) for _ in range(2)]
                STT = [S.sb([128, 128], F32, 'STT', es=ph) for _ in range(4)]
                vps = [S.ps([128, NT], F32, 'vps', es=ph) for _ in range(4)]
                ops = [S.ps([128, NT], F32, 'ops', es=ph) for _ in range(2)]
                nps = S.ps([128, NT], F32, 'nps', es=ph)
                osb = S.sb([128, NT], F32, 'osb', es=ph)
                ofb = S.sb([128, NT], F32, 'ofb', es=ph)
                gsb = S.sb([128, NT], F32, 'gsb', es=ph)
                sqb = S.sb([128, NT], F32, 'sqb', es=ph)
                for hh in range(4):
                    for dd in range(2):
                        for s_ in STT:
                            S.memset(s_.v, 0.0)
                        order = order_f if dd == 0 else order_b
                        for ii, bi in enumerate(order):
                            t0, n = blks[bi]
                            b2 = ii % 2
                            S.dma('sp', dec[b2][:, :n], HF[dd][hh][bi].v)
                            S.dma('pool', qq[b2][:, :n], HQ[hh][bi].v)
                            S.dma('sp', vv[b2][:, :n], HV[hh][bi].v)
                            S.ts(kk[b2][:, :n], dec[b2][:, :n], -1.0, ALU.mult, 1.0, ALU.add, eng='pool')
                            S.copy(vvb[b2][:, :n], vv[b2][:, :n], 'act')
                            op_ = ops[b2]
                            items = []
                            for v in range(128):
                                items.append((v, kk[b2], dec[b2], STT[v % 4][:, v:v + 1],
                                              (lambda pv, v=v: S.mm(op_[:, :n], zselb[:, 127 - v:255 - v], pv,
                                                                    start=(v == 0), stop=(v == 127)))))
                            gla_vloop(S, items, n, dd == 1, identb, vvb[b2], qq[b2], vps, tm, st, pp)
                            if dd == 0:
                                S.copy(osb[:, :n], op_[:, :n], 'act')
                                S.dma('sp', OF[bi].v, osb[:, :n])
                            else:
                                S.dma('sp', ofb[:, :n], OF[bi].v)
                                S.dma('pool', gsb[:, :n], HG[hh][bi].v)
                                S.tt(osb[:, :n], op_[:, :n], ofb[:, :n], ALU.add)
                                S.act(sqb[:, :n], osb[:, :n], AF.Square)
                                S.mm(nps[:, :n], ones.v, sqb[:, :n])
                                S.ts(sqb[:, :n], nps[:, :n], 1.0 / 128, ALU.mult, NORM_EPS, ALU.add)
                                S.act(sqb[:, :n], sqb[:, :n], AF.Sqrt)
                                S.recip(sqb[:, :n], sqb[:, :n])
                                S.tt(osb[:, :n], osb[:, :n], sqb[:, :n], ALU.mult)
                                S.tt(osb[:, :n], osb[:, :n], gsb[:, :n], ALU.mult)
                                S.dma('sp', YS[hh][bi].v, osb[:, :n])
                                if out_hg is not None:
                                    S.dma('sp', out_hg[hh * 128:(hh + 1) * 128, t0:t0 + n], osb[:, :n])
                S.barrier()
            if stage == 'hg':
                break

            with contextlib.ExitStack() as ph:
                NT = 512
                dec = [S.sb([128, NT], F32, 'mdec', es=ph) for _ in range(2)]
                kk = [S.sb([128, NT], F32, 'mkk', es=ph) for _ in range(2)]
                qq = [S.sb([128, NT], F32, 'mqq', es=ph) for _ in range(2)]
                vv = [S.sb([128, NT], F32, 'mvv', es=ph) for _ in range(2)]
                ge = [S.sb([16, NT], F32, 'mge', es=ph) for _ in range(2)]
                gs = [S.sb([16, NT], F32, 'mgs', es=ph) for _ in range(2)]
                tm = [S.sb([128, NT], F32, 'mtm', es=ph) for _ in range(4)]
                st = [S.sb([128, NT], F32, 'mst', es=ph) for _ in range(4)]
                pp = [S.sb([128, NT], BF16, 'mpp', es=ph) for _ in range(4)]
                vvb = [S.sb([128, NT], BF16, 'mvvb', es=ph) for _ in range(2)]
                STT = [S.sb([128, 130], F32, 'mSTT', es=ph) for _ in range(4)]
                vps = [S.ps([128, NT], F32, 'mvps', es=ph) for _ in range(4)]
                ops = [S.ps([128, NT], F32, 'mops', es=ph) for _ in range(2)]
                dps = S.ps([128, NT], F32, 'mdps', es=ph)
                nps = S.ps([128, NT], F32, 'mnps', es=ph)
                osb = S.sb([128, NT], F32, 'mosb', es=ph)
                ofb = S.sb([128, NT], F32, 'mofb', es=ph)
                gsb = S.sb([128, NT], F32, 'mgsb', es=ph)
                sqb = S.sb([128, NT], F32, 'msqb', es=ph)
                dnb = S.sb([128, NT], F32, 'mdnb', es=ph)
                for hh in range(4):
                    for dd in range(2):
                        for s_ in STT:
                            S.memset(s_.v, 0.0)
                        order = order_f if dd == 0 else order_b
                        for ii, bi in enumerate(order):
                            t0, n = blks[bi]
                            b2 = ii % 2
                            S.dma('sp', ge[b2][:, :n], MGE[bi].v)
                            S.dma('pool', gs[b2][:, :n], MGS[bi].v)
                            S.dma('sp', qq[b2][:, :n], MQ[hh][bi].v)
                            S.dma('pool', vv[b2][:, :n], MV[hh][bi].v)
                            S.dma('sp', tm[0][:, :n], MK[hh][bi].v)
                            ri, rf = dd * 4 + hh, 8 + dd * 4 + hh
                            S.mm(vps[0][:, :n], ident[0:16, rf:rf + 1].bc([16, 128]), gs[b2][:, :n])
                            S.copy(dec[b2][:, :n], vps[0][:, :n], 'act')
                            S.mm(vps[1][:, :n], ident[0:16, ri:ri + 1].bc([16, 128]), ge[b2][:, :n])
                            S.tt(kk[b2][:, :n], vps[1][:, :n], tm[0][:, :n], ALU.mult)
                            S.copy(vvb[b2][:, :n], vv[b2][:, :n], 'act')
                            op_ = ops[b2]
                            items = []
                            for v in range(128):
                                items.append((v, kk[b2], dec[b2], STT[v % 4][:, v:v + 1],
                                              (lambda pv, v=v: S.mm(op_[:, :n], zselb[:, 127 - v:255 - v], pv,
                                                                    start=(v == 0), stop=(v == 127)))))
                            items.append((None, kk[b2], dec[b2], STT[0][:, 128:129],
                                          (lambda pv: S.mm(dps[:, :n], onesb.v, pv))))
                            gla_vloop(S, items, n, dd == 1, identb, vvb[b2], qq[b2], vps, tm, st, pp)
                            S.act(dnb[:, :n], dps[:, :n], AF.Abs)
                            S.ts(dnb[:, :n], dnb[:, :n], 1.0, ALU.max)
                            S.recip(dnb[:, :n], dnb[:, :n])
                            if dd == 0:
                                S.tt(osb[:, :n], op_[:, :n], dnb[:, :n], ALU.mult)
                                S.dma('sp', OF[bi].v, osb[:, :n])
                            else:
                                S.dma('sp', ofb[:, :n], OF[bi].v)
                                S.dma('pool', gsb[:, :n], MO[hh][bi].v)
                                S.tt(osb[:, :n], op_[:, :n], dnb[:, :n], ALU.mult)
                                S.tt(osb[:, :n], osb[:, :n], ofb[:, :n], ALU.add)
                                S.act(sqb[:, :n], osb[:, :n], AF.Square)
                                S.mm(nps[:, :n], ones.v, sqb[:, :n])
                                S.ts(sqb[:, :n], nps[:, :n], 1.0 / 128, ALU.mult, NORM_EPS, ALU.add)
                                S.act(sqb[:, :n], sqb[:, :n], AF.Sqrt)
                                S.recip(sqb[:, :n], sqb[:, :n])
                                S.tt(osb[:, :n], osb[:, :n], sqb[:, :n], ALU.mult)
                                S.tt(osb[:, :n], osb[:, :n], gsb[:, :n], ALU.mult)
                                S.dma('sp', YS[12 + hh][bi].v, osb[:, :n])
                                if out_ml is not None:
                                    S.dma('sp', out_ml[hh * 128:(hh + 1) * 128, t0:t0 + n], osb[:, :n])
                S.barrier()
            if stage == 'ml':
                break

            seg_ctx = [i for i in ctx_b]
            seg_lat = [i for i in lat_b]
            with contextlib.ExitStack() as ph:
                xp = S.sb([128, seq + 4], F32, 'cxp', es=ph)
                xo = S.sb([128, seq], F32, 'cxo', es=ph)
                for j in range(8):
                    for seg in (seg_ctx, seg_lat):
                        L = sum(blks[i][1] for i in seg)
                        S.memset(xp[:, 0:2], 0.0)
                        S.memset(xp[:, 2 + L:4 + L], 0.0)
                        off = 0
                        for i in seg:
                            S.dma('sp' if i % 2 else 'pool', xp[:, 2 + off:2 + off + blks[i][1]], SXR[j][i].v)
                            off += blks[i][1]
                        S.ts(xo[:, :L], xp[:, 0:L], scw[:, l, j, 0:1], ALU.mult)
                        for tp in range(1, 4):
                            S.stt(xo[:, :L], xp[:, tp:tp + L], scw[:, l, j, tp:tp + 1], xo[:, :L],
                                  ALU.mult, ALU.add)
                        S.act(xo[:, :L], xo[:, :L], AF.Silu)
                        off = 0
                        for i in seg:
                            S.dma('sp' if i % 2 else 'pool', SXC[j][i].v, xo[:, off:off + blks[i][1]])
                            off += blks[i][1]
                S.barrier()

            if stage == 'ssa':
                break
            with contextlib.ExitStack() as ph:
                NT = 512
                dec = [S.sb([128, NT], F32, 'sdec', es=ph) for _ in range(2)]
                kk = [S.sb([128, NT], F32, 'skk', es=ph) for _ in range(2)]
                qq = [S.sb([128, NT], F32, 'sqq', es=ph) for _ in range(2)]
                bb = [S.sb([128, NT], F32, 'sbb', es=ph) for _ in range(2)]
                vv = [S.sb([128, NT], F32, 'svv', es=ph) for _ in range(2)]
                dl = [S.sb([16, NT], F32, 'sdl', es=ph) for _ in range(2)]
                tm = [S.sb([128, NT], F32, 'stm', es=ph) for _ in range(4)]
                st = [S.sb([128, NT], F32, 'sst', es=ph) for _ in range(4)]
                pp = [S.sb([128, NT], BF16, 'spp', es=ph) for _ in range(4)]
                vvb = [S.sb([128, NT], BF16, 'svvb', es=ph) for _ in range(2)]
                STT = [[S.sb([128, 64], F32, 'sSTT', es=ph) for _ in range(4)] for _ in range(2)]
                vps = [S.ps([128, NT], F32, 'svps', es=ph) for _ in range(4)]
                dps = S.ps([128, NT], F32, 'sdps', es=ph)
                ops = [S.ps([128, NT], F32, 'sops', es=ph) for _ in range(2)]
                osb = S.sb([128, NT], F32, 'sosb', es=ph)
                ofb = S.sb([128, NT], F32, 'sofb', es=ph)
                for ch in range(4):
                    gg = ch // 2
                    for dd in range(2):
                        for hs in range(2):
                            for s_ in STT[hs]:
                                S.memset(s_.v, 0.0)
                        order = order_f if dd == 0 else order_b
                        for ii, bi in enumerate(order):
                            t0, n = blks[bi]
                            b2 = ii % 2
                            S.dma('sp', dl[b2][:, :n], SD[bi].v)
                            S.dma('pool', qq[b2][:, :n], SXC[6 + gg][bi].v)
                            S.dma('sp', bb[b2][:, :n], SXC[4 + gg][bi].v)
                            S.dma('pool', vv[b2][:, :n], SXC[ch][bi].v)
                            S.copy(vvb[b2][:, :n], vv[b2][:, :n], 'act')
                            op_ = ops[b2]
                            items = []
                            for hs in range(2):
                                hh = ch * 2 + hs
                                rr_ = dd * 8 + hh
                                S.mm(dps[:, :n], ident[0:16, rr_:rr_ + 1].bc([16, 128]), dl[b2][:, :n])
                                S.ts(dec[hs][:, :n], dps[:, :n], nega[:, l, rr_:rr_ + 1], ALU.mult)
                                S.act(dec[hs][:, :n], dec[hs][:, :n], AF.Exp)
                                S.tt(kk[hs][:, :n], dps[:, :n], bb[b2][:, :n], ALU.mult)
                                for v in range(64):
                                    row = hs * 64 + v
                                    items.append((row, kk[hs], dec[hs], STT[hs][v % 4][:, v:v + 1],
                                                  (lambda pv, row=row: S.mm(op_[:, :n], zselb[:, 127 - row:255 - row], pv,
                                                                            start=(row == 0), stop=(row == 127)))))
                            gla_vloop(S, items, n, dd == 1, identb, vvb[b2], qq[b2], vps, tm, st, pp)
                            if dd == 0:
                                S.copy(osb[:, :n], op_[:, :n], 'act')
                                S.dma('sp', SY[ch][bi].v, osb[:, :n])
                            else:
                                S.dma('sp', ofb[:, :n], SY[ch][bi].v)
                                S.tt(osb[:, :n], op_[:, :n], ofb[:, :n], ALU.add)
                                S.stt(osb[:, :n], vv[b2][:, :n], dsk[:, l, ch:ch + 1], osb[:, :n],
                                      ALU.mult, ALU.add)
                                S.dma('sp', SY[ch][bi].v, osb[:, :n])
                S.barrier()

            if stage == 'ssb':
                break
            with contextlib.ExitStack() as ph:
                NT = 512
                yb = [S.sb([128, NT], F32, 'fyb', es=ph) for _ in range(2)]
                zb = [S.sb([128, NT], F32, 'fzb', es=ph) for _ in range(2)]
                sq2 = [S.sb([128, NT], F32, 'fsq2', es=ph) for _ in range(2)]
                rs = S.sb([128, NT], F32, 'frs', es=ph)
                nps = S.ps([128, NT], F32, 'fnps', es=ph)
                for bi, (t0, n) in enumerate(blks):
                    for gg in range(2):
                        for cc in range(2):
                            ch = gg * 2 + cc
                            S.dma('pool', yb[cc][:, :n], SY[ch][bi].v)
                            S.dma('sp', zb[cc][:, :n], SZ[ch][bi].v)
                            S.tt(yb[cc][:, :n], yb[cc][:, :n], zb[cc][:, :n], ALU.mult)
                            S.act(sq2[cc][:, :n], yb[cc][:, :n], AF.Square)
                            S.mm(nps[:, :n], ones.v, sq2[cc][:, :n], start=(cc == 0), stop=(cc == 1))
                        S.ts(rs[:, :n], nps[:, :n], 1.0 / 256, ALU.mult, NORM_EPS, ALU.add)
                        S.act(rs[:, :n], rs[:, :n], AF.Sqrt)
                        S.recip(rs[:, :n], rs[:, :n])
                        for cc in range(2):
                            ch = gg * 2 + cc
                            S.tt(yb[cc][:, :n], yb[cc][:, :n], rs[:, :n], ALU.mult)
                            S.ts(yb[cc][:, :n], yb[cc][:, :n], sng[:, l, ch:ch + 1], ALU.mult)
                            S.dma('sp', YS[4 + ch][bi].v, yb[cc][:, :n])
                            if out_ss is not None:
                                S.dma('sp', out_ss[ch * 128:(ch + 1) * 128, t0:t0 + n], yb[cc][:, :n])
                S.barrier()
            if stage == 'ss':
                break

            with contextlib.ExitStack() as ph:
                NT = 512
                GW = 64
                RR = seq // GW
                raw = S.sb([128, seq], F32, 'lraw', es=ph)
                xpd = S.sb([128, seq + 4], F32, 'lxpd', es=ph)
                xcv = S.sb([128, seq], F32, 'lxcv', es=ph)
                rawc = S.sb([128, ctx], F32, 'lrawc', es=ph)
                xpc = S.sb([128, ctx + 4], F32, 'lxpc', es=ph)
                xcc = S.sb([128, ctx], F32, 'lxcc', es=ph)
                wa_t = [S.sb([128, 128], F32, 'lwa', es=ph) for _ in range(2)]
                wx_t = [S.sb([128, 128], F32, 'lwx', es=ph) for _ in range(2)]
                rt = S.sb([128, NT], F32, 'lrt', es=ph)
                it = S.sb([128, NT], F32, 'lit', es=ph)
                at = S.sb([128, NT], F32, 'lat', es=ph)
                mt = S.sb([128, NT], F32, 'lmt', es=ph)
                bt = S.sb([128, NT], F32, 'lbt', es=ph)
                ht = S.sb([128, NT], F32, 'lht', es=ph)
                gt = [S.sb([128, NT], F32, 'lgt', es=ph) for _ in range(2)]
                hst = [S.sb([128, 1], F32, 'lhst', es=ph) for _ in range(2)]
                rps = S.ps([128, NT], F32, 'lrps', es=ph)
                ips = S.ps([128, NT], F32, 'lips', es=ph)
                S.memset(xpd[:, 0:2], 0.0)
                S.memset(xpd[:, 2 + seq:4 + seq], 0.0)
                S.memset(xpc[:, 0:2], 0.0)
                S.memset(xpc[:, 2 + ctx:4 + ctx], 0.0)
                pctx = [(s, min(NT, ctx - s)) for s in range(0, ctx, NT)]
                plat = [(s, min(NT, seq - s)) for s in range(0, seq, NT)]
                for j in range(4):
                    for i in ctx_b:
                        S.dma('sp', rawc[:, blks[i][0]:blks[i][0] + blks[i][1]], LXR[j][i].v)
                    for i in lat_b:
                        S.dma('sp' if i % 2 else 'pool', raw[:, blks[i][0] - ctx:blks[i][0] - ctx + blks[i][1]],
                              LXR[j][i].v)
                    S.copy(xpc[:, 2:2 + ctx], rawc.v, 'act')
                    S.copy(xpd[:, 2:2 + seq].rr("p (c r) -> p c r", c=GW), raw.v.rr("p (r c) -> p c r", c=GW))
                    for (xo_, xp_, L) in ((xcc, xpc, ctx), (xcv, xpd, seq)):
                        S.ts(xo_[:, :L], xp_[:, 0:L], lcw[:, l, j, 0:1], ALU.mult)
                        for tp in range(1, 4):
                            S.stt(xo_[:, :L], xp_[:, tp:tp + L], lcw[:, l, j, tp:tp + 1], xo_[:, :L],
                                  ALU.mult, ALU.add)
                    for dd in range(2):
                        S.dma('sp', wa_t[dd].v, lwa_in[l, dd, j])
                        S.dma('pool', wx_t[dd].v, lwx_in[l, dd, j])
                    for dd in range(2):
                        S.memset(hst[0].v, 0.0)
                        S.memset(hst[1].v, 0.0)
                        segs = [(xcc, None, pctx), (xcv, raw, plat)]
                        cnt = 0
                        for (xsrc, hdst, pb) in segs:
                            for (s0, n) in (pb if dd == 0 else pb[::-1]):
                                xb_ = xsrc[:, s0:s0 + n]
                                S.mm(rps[:, :n], wa_t[dd].v, xb_)
                                S.mm(ips[:, :n], wx_t[dd].v, xb_)
                                S.act(rt[:, :n], rps[:, :n], AF.Sigmoid, bias=lba[:, l, dd, j:j + 1])
                                S.act(it[:, :n], ips[:, :n], AF.Sigmoid, bias=lbx[:, l, dd, j:j + 1])
                                S.ts(at[:, :n], rt[:, :n], CL[:, l, dd, j:j + 1], ALU.mult)
                                S.act(at[:, :n], at[:, :n], AF.Exp)
                                S.tt(mt[:, :n], at[:, :n], at[:, :n], ALU.mult)
                                S.ts(mt[:, :n], mt[:, :n], -1.0, ALU.mult, 1.0, ALU.add)
                                S.act(mt[:, :n], mt[:, :n], AF.Sqrt)
                                S.tt(bt[:, :n], it[:, :n], xb_, ALU.mult, eng='pool')
                                S.tt(bt[:, :n], bt[:, :n], mt[:, :n], ALU.mult)
                                c2 = cnt % 2
                                if dd == 0:
                                    S.scan(ht[:, :n], at[:, :n], bt[:, :n], hst[c2].v)
                                    S.copy(hst[1 - c2].v, ht[:, n - 1:n], 'act')
                                else:
                                    S.scan(ht[:, :n][:, ::-1], at[:, :n][:, ::-1], bt[:, :n][:, ::-1], hst[c2].v)
                                    S.copy(hst[1 - c2].v, ht[:, 0:1], 'act')
                                cnt += 1
                                hd_ = (rawc if hdst is None else raw)[:, s0:s0 + n]
                                if dd == 0:
                                    S.copy(hd_, ht[:, :n], 'pool')
                                else:
                                    S.tt(hd_, hd_, ht[:, :n], ALU.add, eng='pool')
                    S.copy(xpd[:, 2:2 + seq].rr("p (r c) -> p c r", c=GW), raw.v.rr("p (c r) -> p c r", c=GW))
                    for i in ctx_b + lat_b:
                        t0, n = blks[i]
                        g_ = gt[i % 2]
                        S.dma('sp', g_[:, :n], LG[j][i].v)
                        src_ = rawc[:, t0:t0 + n] if t0 < ctx else xpd[:, 2 + t0 - ctx:2 + t0 - ctx + n]
                        S.tt(g_[:, :n], g_[:, :n], src_, ALU.mult)
                        S.dma('sp', YS[8 + j][i].v, g_[:, :n])
                        if out_lr is not None:
                            S.dma('sp', out_lr[j * 128:(j + 1) * 128, t0:t0 + n], g_[:, :n])
                S.barrier()
            if stage == 'lr':
                break

            with contextlib.ExitStack() as ph:
                NT = 512
                wob = S.sb([128, KC, D], BF16, 'wob', es=ph)
                wos = [S.sb([128, 2, D], F32, 'wos', es=ph) for _ in range(2)]
                for q8 in range(8):
                    w_ = wos[q8 % 2]
                    S.dma('sp' if q8 % 2 else 'pool', w_.v,
                          w_out_in[l, q8 * 256:(q8 + 1) * 256, :].rearrange("(k p) c -> p k c", p=128))
                    S.copy(wob[:, q8 * 2:(q8 + 1) * 2, :], w_.v, 'act' if q8 % 2 else 'dve')
                yst = S.sb([128, KC, NT], F32, 'yst', es=ph)
                ybf = S.sb([128, KC, NT], BF16, 'ybf', es=ph)
                xt = [S.sb([128, NT], F32, 'xt', es=ph) for _ in range(2)]
                xn = [S.sb([128, NT], F32, 'xn', es=ph) for _ in range(2)]
                yps = [S.ps([128, NT], F32, 'yps', es=ph) for _ in range(2)]
                for bi, (t0, n) in enumerate(blks):
                    j = 1 if t0 < ctx else 0
                    for k in range(KC):
                        S.dma('sp' if k % 2 else 'pool', yst[:, k, :n], YS[k][bi].v)
                    S.copy(ybf[:, 0:8, :n], yst[:, 0:8, :n], 'dve')
                    S.copy(ybf[:, 8:16, :n], yst[:, 8:16, :n], 'act')
                    for oc in range(KC):
                        o2 = oc % 2
                        S.dma('sp', xt[o2][:, :n], X[bi][oc * 128:(oc + 1) * 128, :])
                        for k in range(KC):
                            S.mm(yps[o2][:, :n], wob[:, k, oc * 128:(oc + 1) * 128], ybf[:, k, :n],
                                 start=(k == 0), stop=(k == KC - 1))
                        S.stt(xn[o2][:, :n], yps[o2][:, :n], MOD[:, 32 + oc, j:j + 1], xt[o2][:, :n],
                              ALU.mult, ALU.add)
                        S.dma('pool', X[bi][oc * 128:(oc + 1) * 128, :], xn[o2][:, :n])
                        if out_xm is not None:
                            S.dma('sp', out_xm[oc * 128:(oc + 1) * 128, t0:t0 + n], xn[o2][:, :n])
                S.barrier()
            if stage == 'mix':
                break

            with contextlib.ExitStack() as ph:
                NT = 512
                rw = S.sb([128, KC, 36], F32, 'rw', es=ph)
                S.dma('sp', rw.v, rw_in[l])
                xb = S.sb([128, KC, NT], F32, 'mxb', es=ph)
                h2b = S.sb([128, KC, NT], BF16, 'h2b', es=ph)
                rstd = S.sb([128, NT], F32, 'mrstd', es=ph)
                tmp = S.sb([128, NT], F32, 'mtmp', es=ph)
                wgb = S.sb([128, KC, 512], BF16, 'wgb', es=ph)
                wub = S.sb([128, KC, 512], BF16, 'wub', es=ph)
                wdb = S.sb([128, 4, D], BF16, 'wdb', es=ph)
                stg = [S.sb([128, 4, 512], F32, 'stg', es=ph) for _ in range(3)]
                hid = S.sb([128, 4, NT], BF16, 'hid', es=ph)
                wbc = S.sb([128, NT], F32, 'wbc', es=ph)
                sg = [S.sb([128, NT], F32, 'sg', es=ph) for _ in range(2)]
                cwt = S.sb([32, NT], F32, 'cwt', es=ph)
                cwp = S.sb([128, 128], F32, 'cwp', es=ph)
                S.memset(cwp.v, 0.0)
                xt = [S.sb([128, NT], F32, 'mxt', es=ph) for _ in range(2)]
                lg = S.sb([128, 36], F32, 'lg', es=ph)
                r4 = [S.sb([128, 4], F32, 'r4', es=ph) for _ in range(3)]
                r1 = [S.sb([128, 1], F32, 'r1', es=ph) for _ in range(8)]
                e32 = [S.sb([128, 32], F32, 'e32', es=ph) for _ in range(5)]
                ssp = S.ps([128, NT], F32, 'mssp', es=ph)
                rps = S.ps([128, NT], F32, 'mrps', es=ph)
                gps = [S.ps([128, NT], F32, 'gps', es=ph) for _ in range(2)]
                ups = [S.ps([128, NT], F32, 'mups', es=ph) for _ in range(2)]
                dps = [S.ps([128, NT], F32, 'mdps', es=ph) for _ in range(2)]
                ceng = ('dve', 'act', 'pool')
                WGS = [S.dram([128, KC, 512], BF16, 'WGS') for _ in range(32)]
                WUS = [S.dram([128, KC, 512], BF16, 'WUS') for _ in range(32)]
                WDS = [S.dram([128, 4, D], BF16, 'WDS') for _ in range(32)]
                nproc = 0
                for bi, (t0, n) in enumerate(blks):
                    if l == depth - 1 and t0 < ctx:
                        continue
                    wcached = nproc > 0
                    nproc += 1
                    j = 1 if t0 < ctx else 0
                    for q4 in range(4):
                        S.dma('sp' if q4 % 2 == 0 else 'pool', xb[:, q4 * 4:(q4 + 1) * 4, :n],
                              X[bi][q4 * 512:(q4 + 1) * 512, :].rr("(k p) t -> p k t", p=128))
                    for k in range(KC):
                        S.act(tmp[:, :n], xb[:, k, :n], AF.Square)
                        S.mm(ssp[:, :n], ones.v, tmp[:, :n], start=(k == 0), stop=(k == KC - 1))
                    S.ts(tmp[:, :n], ssp[:, :n], 1.0 / D, ALU.mult, NORM_EPS, ALU.add)
                    S.act(tmp[:, :n], tmp[:, :n], AF.Sqrt)
                    S.recip(rstd[:, :n], tmp[:, :n])
                    for k in range(KC):
                        S.tt(xb[:, k, :n], xb[:, k, :n], rstd[:, :n], ALU.mult)
                        S.ts(xb[:, k, :n], xb[:, k, :n], A2[:, k, j:j + 1], ALU.mult,
                             MOD[:, 48 + k, j:j + 1], ALU.add)
                        S.copy(h2b[:, k, :n], xb[:, k, :n], 'act' if k % 2 else 'pool')
                    for s in range(0, n, 128):
                        ns = min(128, n - s)
                        for k in range(KC):
                            S.mm(rps[:ns, 0:36], xb[:, k, s:s + ns], rw[:, k, :], start=(k == 0), stop=(k == KC - 1))
                        S.copy(lg[:ns, :], rps[:ns, 0:36], 'act')
                        gmax, gsum, gp, m1, m2, dm, w1, w2 = [t_[:ns, :] for t_ in r1]
                        ge, ohg, pen = [t_[:ns, :] for t_ in r4]
                        elm, oh1, elm2, oh2, cw = [t_[:ns, :] for t_ in e32]
                        S.red(gmax, lg[:ns, 0:4], ALU.max)
                        S.ts(ge, lg[:ns, 0:4], gmax, ALU.subtract)
                        S.act(ge, ge, AF.Exp)
                        S.red(gsum, ge, ALU.add)
                        S.recip(gp, gsum)
                        S.ts(ohg, lg[:ns, 0:4], gmax, ALU.is_equal)
                        S.ts(pen, ohg, -1.0, ALU.add, 1e30, ALU.mult)
                        S.tt(elm.rr("p (g e) -> p g e", g=4), lg[:ns, 4:36].rr("p (g e) -> p g e", g=4),
                             pen.rr("p (g o) -> p g o", o=1).bc([ns, 4, 8]), ALU.add)
                        S.red(m1, elm, ALU.max)
                        S.ts(oh1, elm, m1, ALU.is_equal)
                        S.stt(elm2, oh1, -1e30, elm, ALU.mult, ALU.add)
                        S.red(m2, elm2, ALU.max)
                        S.ts(oh2, elm2, m2, ALU.is_equal)
                        S.tt(dm, m2, m1, ALU.subtract)
                        S.act(dm, dm, AF.Exp)
                        S.ts(w1, dm, 1.0, ALU.add)
                        S.recip(w1, w1)
                        S.tt(w2, dm, w1, ALU.mult)
                        S.tt(w1, w1, gp, ALU.mult)
                        S.tt(w2, w2, gp, ALU.mult)
                        S.ts(cw, oh1, w1, ALU.mult)
                        S.stt(cwp[:ns, 0:32], oh2, w2, cw, ALU.mult, ALU.add)
                        S.tr(rps[:, 128:128 + ns], cwp[:ns, :], ident[:ns, :ns])
                        S.copy(cwt[:, s:s + ns], rps[:32, 128:128 + ns], 'act')
                    for e in range(32):
                        if not wcached:
                            for q4 in range(4):
                                st_ = stg[0]
                                S.dma('sp', st_.v, wg_in[l, e, q4 * 512:(q4 + 1) * 512, :].rearrange("(k p) c -> p k c", p=128))
                                S.copy(wgb[:, q4 * 4:(q4 + 1) * 4, :], st_.v, ceng[q4 % 3])
                                st_ = stg[1]
                                S.dma('pool', st_.v, wu_in[l, e, q4 * 512:(q4 + 1) * 512, :].rearrange("(k p) c -> p k c", p=128))
                                S.copy(wub[:, q4 * 4:(q4 + 1) * 4, :], st_.v, ceng[(q4 + 1) % 3])
                                st_ = stg[2]
                                S.dma('sp', st_.v.rr("p a c -> p (a c)"), wd_in[l, e, q4 * 128:(q4 + 1) * 128, :])
                                S.copy(wdb[:, q4, :], st_.v.rr("p a c -> p (a c)"), ceng[(q4 + 2) % 3])
                            S.dma('sp', WGS[e].v, wgb.v)
                            S.dma('pool', WUS[e].v, wub.v)
                            S.dma('sp', WDS[e].v, wdb.v)
                        else:
                            S.dma('sp', wgb.v, WGS[e].v)
                            S.dma('pool', wub.v, WUS[e].v)
                            S.dma('sp', wdb.v, WDS[e].v)
                        S.mm(rps[:, :n], ident[0:32, e:e + 1].bc([32, 128]), cwt[:, :n])
                        S.copy(wbc[:, :n], rps[:, :n], 'act')
                        for f in range(4):
                            f2 = f % 2
                            for k in range(KC):
                                S.mm(gps[f2][:, :n], wgb[:, k, f * 128:(f + 1) * 128], h2b[:, k, :n],
                                     start=(k == 0), stop=(k == KC - 1))
                            for k in range(KC):
                                S.mm(ups[f2][:, :n], wub[:, k, f * 128:(f + 1) * 128], h2b[:, k, :n],
                                     start=(k == 0), stop=(k == KC - 1))
                            S.act(sg[f2][:, :n], gps[f2][:, :n], AF.Silu)
                            S.tt(sg[f2][:, :n], sg[f2][:, :n], ups[f2][:, :n], ALU.mult)
                            S.tt(hid[:, f, :n], sg[f2][:, :n], wbc[:, :n], ALU.mult, eng='pool')
                        for oc in range(KC):
                            o2 = oc % 2
                            for f in range(4):
                                S.mm(dps[o2][:, :n], wdb[:, f, oc * 128:(oc + 1) * 128], hid[:, f, :n],
                                     start=(f == 0), stop=(f == 3))
                            if e == 0:
                                S.copy(xb[:, oc, :n], dps[o2][:, :n], 'act' if o2 else 'dve')
                            else:
                                S.tt(xb[:, oc, :n], xb[:, oc, :n], dps[o2][:, :n], ALU.add)
                    for oc in range(KC):
                        o2 = oc % 2
                        S.dma('sp', xt[o2][:, :n], X[bi][oc * 128:(oc + 1) * 128, :])
                        S.stt(xt[o2][:, :n], xb[:, oc, :n], MOD[:, 80 + oc, j:j + 1], xt[o2][:, :n],
                              ALU.mult, ALU.add)
                        S.dma('pool', X[bi][oc * 128:(oc + 1) * 128, :], xt[o2][:, :n])
                S.barrier()

        with contextlib.ExitStack() as ph:
            NT = 512
            fg = S.sb([128, KC], F32, 'fg', es=ph)
            S.dma('sp', fg.v, fng)
            xb = [S.sb([128, KC, NT], F32, 'fxb', es=ph) for _ in range(2)]
            sq = S.sb([128, KC, NT], F32, 'fsq', es=ph)
            rstd = S.sb([128, NT], F32, 'frstd', es=ph)
            tmp = S.sb([128, NT], F32, 'ftmp', es=ph)
            ssp = S.ps([128, NT], F32, 'fssp', es=ph)
            for bi, (t0, n) in enumerate(blks):
                if t0 < ctx:
                    continue
                x_ = xb[bi % 2]
                for q4 in range(4):
                    S.dma('sp' if q4 % 2 == 0 else 'pool', x_[:, q4 * 4:(q4 + 1) * 4, :n],
                          X[bi][q4 * 512:(q4 + 1) * 512, :].rr("(k p) t -> p k t", p=128))
                S.act(sq[:, :, :n], x_[:, :, :n], AF.Square)
                for k in range(KC):
                    S.mm(ssp[:, :n], ones.v, sq[:, k, :n], start=(k == 0), stop=(k == KC - 1))
                S.ts(tmp[:, :n], ssp[:, :n], 1.0 / D, ALU.mult, NORM_EPS, ALU.add)
                S.act(tmp[:, :n], tmp[:, :n], AF.Sqrt)
                S.recip(rstd[:, :n], tmp[:, :n])
                for k in range(KC):
                    S.tt(sq[:, k, :n], x_[:, k, :n], rstd[:, :n], ALU.mult)
                    S.ts(sq[:, k, :n], sq[:, k, :n], fg[:, k:k + 1], ALU.mult)
                for q4 in range(4):
                    S.dma('sp' if q4 % 2 == 0 else 'pool',
                          outT[q4 * 512:(q4 + 1) * 512, t0 - ctx:t0 - ctx + n].rearrange("(k p) t -> p k t", p=128),
                          sq[:, q4 * 4:(q4 + 1) * 4, :n])
            S.barrier()
        S.finish()
    return nc


def _cols(v, nchunk):
    return np.ascontiguousarray(np.asarray(v, np.float32).reshape(nchunk, 128).T)


def _lru_cols(v, depth):
    return np.ascontiguousarray(np.asarray(v, np.float32)[:depth].reshape(depth, 2, 4, 128).transpose(3, 0, 1, 2))


def _blockdiag(w, depth):
    w = np.asarray(w, np.float32)[:depth]
    out = np.zeros((depth, 2, 4, 128, 128), np.float32)
    for j in range(4):
        out[:, :, j, 0:64, 0:64] = w[:, :, 2 * j]
        out[:, :, j, 64:128, 64:128] = w[:, :, 2 * j + 1]
    return out


def make_inputs(b, depth, x, c, ctx, c_ctx, ada_w, ada_b, norm1_g, norm2_g, w_in, final_norm_g, w_out,
                hg_lb_logits, ml_b_i, ml_b_f, ssd_conv_w, ssd_a_log, ssd_dt_bias, ssd_d, ssd_norm_g,
                lru_conv_w, lru_wa, lru_ba, lru_wx, lru_bx, lru_lambda,
                router_group_w, router_expert_w, moe_w_gate, moe_w_up, moe_w_down, **_):
    xT = np.ascontiguousarray(np.concatenate([ctx[b], x[b]], axis=0).T.astype(np.float32))
    sv = np.stack([_cols(c[b], KC), _cols(c_ctx, KC)], axis=-1)
    return {
        "xT": xT, "sv": np.ascontiguousarray(sv),
        "ada_w": np.ascontiguousarray(ada_w[:depth]),
        "ada_b": np.stack([_cols(ada_b[l], 96) for l in range(depth)]),
        "n1g": np.stack([_cols(norm1_g[l], KC) for l in range(depth)]),
        "n2g": np.stack([_cols(norm2_g[l], KC) for l in range(depth)]),
        "fng": _cols(final_norm_g, KC),
        "w_in": np.ascontiguousarray(w_in[:depth]),
        "ident": np.eye(128, dtype=np.float32),
        "rw": np.ascontiguousarray(np.concatenate(
            [np.asarray(router_group_w, np.float32)[:depth], np.asarray(router_expert_w, np.float32)[:depth]],
            axis=2).reshape(depth, KC, 128, 36).transpose(0, 2, 1, 3)),
        "moe_w_gate": np.ascontiguousarray(np.asarray(moe_w_gate, np.float32)[:depth]),
        "moe_w_up": np.ascontiguousarray(np.asarray(moe_w_up, np.float32)[:depth]),
        "moe_w_down": np.ascontiguousarray(np.asarray(moe_w_down, np.float32)[:depth]),
        "w_out": np.ascontiguousarray(np.asarray(w_out, np.float32)[:depth]),
        "lcw": np.ascontiguousarray(np.asarray(lru_conv_w, np.float32)[:depth]
                                    .reshape(depth, 4, 4, 128).transpose(3, 0, 2, 1)),
        "lam": _lru_cols(lru_lambda, depth), "lba": _lru_cols(lru_ba, depth), "lbx": _lru_cols(lru_bx, depth),
        "lwa": _blockdiag(lru_wa, depth), "lwx": _blockdiag(lru_wx, depth),
        "scw": np.ascontiguousarray(np.asarray(ssd_conv_w, np.float32)[:depth]
                                    .reshape(depth, 4, 8, 128).transpose(3, 0, 2, 1)),
        "alog": np.ascontiguousarray(np.broadcast_to(
            np.asarray(ssd_a_log, np.float32)[:depth].reshape(1, depth, 16), (128, depth, 16))),
        "dtb": np.ascontiguousarray(np.asarray(ssd_dt_bias, np.float32)[:depth].reshape(depth, 16).T),
        "dsk": np.ascontiguousarray(np.repeat(np.asarray(ssd_d, np.float32)[:depth], 64, axis=1)
                                    .reshape(depth, 4, 128).transpose(2, 0, 1)),
        "sng": np.ascontiguousarray(np.asarray(ssd_norm_g, np.float32)[:depth]
                                    .reshape(depth, 4, 128).transpose(2, 0, 1)),
        "mlb": np.ascontiguousarray(np.concatenate(
            [np.asarray(ml_b_i, np.float32)[:depth].reshape(depth, 8),
             np.asarray(ml_b_f, np.float32)[:depth].reshape(depth, 8)], axis=1).T),
        "lbl": np.ascontiguousarray(np.asarray(hg_lb_logits, np.float32)[:depth]
                                    .reshape(depth, 2, 4, 128).transpose(3, 1, 2, 0).reshape(128, 8, depth)),
    }


def kernel(**inputs):
    inputs = {k: np.asarray(v) for k, v in inputs.items()}
    x = inputs["x"]
    B, seq, _ = x.shape
    ctx = inputs["ctx"].shape[1]
    depth = inputs["w_in"].shape[0]
    nc = build(depth, ctx, seq)
    in_maps = [make_inputs(b, depth, **inputs) for b in range(B)]
    res = run_bass_kernel_spmd(nc, in_maps, core_ids=list(range(B)))
    out = np.stack([res.results[b]["outT"].T for b in range(B)], axis=0)
    return np.ascontiguousarray(out.astype(np.float32))
```
